# Optimizing a Trainium2 kernel written in Bass

```python
import jax, jax.numpy as jnp
from jax import lax
import numpy as np

D_MODEL = 2048
BATCH = 4
SEQ = 4096
DEPTH = 4

CHUNK = 64
N_A_LAYERS = DEPTH // 2
N_B_LAYERS = DEPTH - N_A_LAYERS
HG_KDIM = 128
HG_HEADS = D_MODEL // HG_KDIM
HG_VDIM = D_MODEL // HG_HEADS
HG_FDIM = HG_HEADS * HG_KDIM
HG_BLOCK = CHUNK // 4
FOX_HDIM = 128
FOX_HEADS = D_MODEL // FOX_HDIM
Q_BLOCK = 128
FORGET_BIAS_INIT = 2.0
EPS = 1e-6

kernel_name = "hgrn2_fox_yoco_streaming_trunk"


def rmsnorm(x, w):
    xf = x.astype(jnp.float32)
    y = xf * lax.rsqrt(jnp.mean(xf * xf, axis=-1, keepdims=True) + EPS)
    return (y * w.astype(jnp.float32)).astype(x.dtype)


def head_rmsnorm(x, w):
    xf = x.astype(jnp.float32)
    return xf * lax.rsqrt(jnp.mean(xf * xf, axis=-1, keepdims=True) + EPS) * w.astype(jnp.float32)


def hgrn2_chunkwise(q, k, v, g):
    B, H, T, dk = q.shape
    dv = v.shape[-1]
    n = T // HG_BLOCK

    def to_blocks(a):
        return jnp.moveaxis(a.reshape(B, H, n, HG_BLOCK, a.shape[-1]), 2, 0)

    mask = jnp.tril(jnp.ones((HG_BLOCK, HG_BLOCK), dtype=bool))[:, :, None]

    def step(S, blk):
        qc, kc, vc, gc = blk
        b = jnp.cumsum(gc, axis=-2)
        diff = b[..., :, None, :] - b[..., None, :, :]
        decay = jnp.exp(jnp.where(mask, diff, -jnp.inf))
        A = jnp.einsum('bhtd,bhsd,bhtsd->bhts', qc, kc, decay)
        o = (jnp.einsum('bhts,bhse->bhte', A, vc)
             + jnp.einsum('bhtd,bhde->bhte', qc * jnp.exp(b), S))
        b_last = b[..., -1:, :]
        S = (jnp.exp(b_last[..., 0, :])[..., None] * S
             + jnp.einsum('bhsd,bhse->bhde', kc * jnp.exp(b_last - b), vc))
        return S, o

    S0 = jnp.zeros((B, H, dk, dv), jnp.float32)
    _, o = lax.scan(step, S0, (to_blocks(q), to_blocks(k), to_blocks(v), to_blocks(g)))
    return jnp.moveaxis(o, 0, 2).reshape(B, H, T, dv)


def hgrn2_layer(x, norm_w, w_in, lb, out_norm_w, w_out):
    B, T, _ = x.shape
    h = rmsnorm(x, norm_w)
    proj = h @ w_in
    q = proj[..., :HG_FDIM]
    fz = proj[..., HG_FDIM:2 * HG_FDIM]
    i = proj[..., 2 * HG_FDIM:2 * HG_FDIM + D_MODEL]
    gate = proj[..., 2 * HG_FDIM + D_MODEL:]
    heads_k = lambda a: a.reshape(B, T, HG_HEADS, HG_KDIM).astype(jnp.float32).transpose(0, 2, 1, 3)
    q, fz = heads_k(q), heads_k(fz)
    i = i.reshape(B, T, HG_HEADS, HG_VDIM).astype(jnp.float32).transpose(0, 2, 1, 3)
    lb = lb.reshape(HG_HEADS, 1, HG_KDIM)
    log_f = jnp.logaddexp(jnp.log(lb), jnp.log1p(-lb) + jax.nn.log_sigmoid(fz))
    k = (1.0 - lb) * jax.nn.sigmoid(-fz)
    o = hgrn2_chunkwise(q, k, i, log_f)
    o = head_rmsnorm(o.transpose(0, 2, 1, 3), out_norm_w.reshape(HG_HEADS, HG_VDIM))
    o = o.reshape(B, T, D_MODEL) * jax.nn.silu(gate.astype(jnp.float32))
    return x + (o.astype(x.dtype) @ w_out)


def shared_kv(x, norm_w, w_kvf, f_bias, k_norm_w):
    B, T, _ = x.shape
    h = rmsnorm(x, norm_w)
    kvf = h @ w_kvf
    k = kvf[..., :D_MODEL].reshape(B, T, FOX_HEADS, FOX_HDIM)
    v = kvf[..., D_MODEL:2 * D_MODEL].reshape(B, T, FOX_HEADS, FOX_HDIM)
    fz = kvf[..., 2 * D_MODEL:].astype(jnp.float32) + f_bias.astype(jnp.float32)
    k = head_rmsnorm(k, k_norm_w).transpose(0, 2, 1, 3)
    v = v.astype(jnp.float32).transpose(0, 2, 1, 3)
    F = jnp.cumsum(jax.nn.log_sigmoid(fz), axis=1).transpose(0, 2, 1)
    return k, v, F


def forgetting_attention(q, k, v, F):
    T = q.shape[2]
    scale = FOX_HDIM ** -0.5
    local = jnp.arange(Q_BLOCK)
    outs = []
    for blk in range(T // Q_BLOCK):
        q0 = blk * Q_BLOCK
        kend = q0 + Q_BLOCK
        s = (jnp.einsum('bhqd,bhkd->bhqk', q[:, :, q0:kend], k[:, :, :kend]) * scale
             + F[:, :, q0:kend, None] - F[:, :, None, :kend])
        causal = (q0 + local)[:, None] >= jnp.arange(kend)[None, :]
        p = jax.nn.softmax(jnp.where(causal, s, -jnp.inf), axis=-1)
        outs.append(jnp.einsum('bhqk,bhkd->bhqd', p, v[:, :, :kend]))
    return jnp.concatenate(outs, axis=2)


def fox_layer(x, norm_w, w_in, q_norm_w, out_norm_w, w_out, k, v, F):
    B, T, _ = x.shape
    h = rmsnorm(x, norm_w)
    proj = h @ w_in
    q = proj[..., :D_MODEL].reshape(B, T, FOX_HEADS, FOX_HDIM)
    gate = proj[..., D_MODEL:]
    q = head_rmsnorm(q, q_norm_w).transpose(0, 2, 1, 3)
    o = forgetting_attention(q, k, v, F)
    o = head_rmsnorm(o.transpose(0, 2, 1, 3), out_norm_w.reshape(FOX_HEADS, FOX_HDIM))
    o = o.reshape(B, T, D_MODEL) * jax.nn.silu(gate.astype(jnp.float32))
    return x + (o.astype(x.dtype) @ w_out)


def setup_inputs(seed: int = 0) -> dict:
    key = jax.random.key(seed)
    ks = jax.random.split(key, 16)
    D = D_MODEL
    s = D ** -0.5
    nrm = jax.random.normal
    return {
        "x": nrm(ks[0], (BATCH, SEQ, D), jnp.float32),
        "a_norm_w": 1.0 + 0.02 * nrm(ks[1], (N_A_LAYERS, D), jnp.float32),
        "a_w_in": s * nrm(ks[2], (N_A_LAYERS, D, 2 * HG_FDIM + 2 * D), jnp.float32),
        "a_lb_logits": 0.1 * nrm(ks[3], (N_A_LAYERS, HG_FDIM), jnp.float32),
        "a_out_norm_w": 1.0 + 0.02 * nrm(ks[4], (N_A_LAYERS, D), jnp.float32),
        "a_w_out": s * nrm(ks[5], (N_A_LAYERS, D, D), jnp.float32),
        "kv_norm_w": 1.0 + 0.02 * nrm(ks[6], (D,), jnp.float32),
        "kv_w": s * nrm(ks[7], (D, 2 * D + FOX_HEADS), jnp.float32),
        "kv_f_bias": FORGET_BIAS_INIT + 0.1 * nrm(ks[8], (FOX_HEADS,), jnp.float32),
        "k_norm_w": 1.0 + 0.02 * nrm(ks[9], (FOX_HDIM,), jnp.float32),
        "b_norm_w": 1.0 + 0.02 * nrm(ks[10], (N_B_LAYERS, D), jnp.float32),
        "b_w_in": s * nrm(ks[11], (N_B_LAYERS, D, 2 * D), jnp.float32),
        "b_q_norm_w": 1.0 + 0.02 * nrm(ks[12], (N_B_LAYERS, FOX_HDIM), jnp.float32),
        "b_out_norm_w": 1.0 + 0.02 * nrm(ks[13], (N_B_LAYERS, D), jnp.float32),
        "b_w_out": s * nrm(ks[14], (N_B_LAYERS, D, D), jnp.float32),
    }


def reference(x, a_norm_w, a_w_in, a_lb_logits, a_out_norm_w, a_w_out,
              kv_norm_w, kv_w, kv_f_bias, k_norm_w,
              b_norm_w, b_w_in, b_q_norm_w, b_out_norm_w, b_w_out):
    lb_all = jnp.cumsum(jax.nn.softmax(a_lb_logits.astype(jnp.float32), axis=0), axis=0)
    lb_all = lb_all - lb_all[0:1]
    k = v = F = None
    for layer in range(DEPTH):
        if layer < N_A_LAYERS:
            x = hgrn2_layer(x, a_norm_w[layer], a_w_in[layer], lb_all[layer],
                            a_out_norm_w[layer], a_w_out[layer])
        else:
            if layer == N_A_LAYERS:
                k, v, F = shared_kv(x, kv_norm_w, kv_w, kv_f_bias, k_norm_w)
            j = layer - N_A_LAYERS
            x = fox_layer(x, b_norm_w[j], b_w_in[j], b_q_norm_w[j], b_out_norm_w[j],
                          b_w_out[j], k, v, F)
    return x
```

```python
import contextlib
import numpy as np
import ml_dtypes
import concourse.bass as bass
import concourse.mybir as mybir
from concourse.bass_utils import run_bass_kernel_spmd

F32 = mybir.dt.float32
BF16 = mybir.dt.bfloat16
ALU = mybir.AluOpType
AF = mybir.ActivationFunctionType

D = 2048
NCH = 16
NH = 16
TB = 512
EPS = 1e-6
CH = 32
NBF = ml_dtypes.bfloat16


class Sched:
    def __init__(self, nc, stack, ndma=40):
        self.nc = nc
        self.engs = {"pe": nc.tensor, "act": nc.scalar, "dve": nc.vector,
                     "pool": nc.gpsimd, "sp": nc.sync}
        self.ops = {e: [] for e in self.engs}
        self.cnt = {e: 0 for e in self.engs}
        self.seen = {e: {} for e in self.engs}
        self.lastw = {}
        self.readers = {}
        self.sem = {}
        for e in self.engs:
            self.sem[e] = stack.enter_context(nc.semaphore("s_" + e))
        self.ndma = ndma
        for k in range(ndma):
            self.sem[("dma", k)] = stack.enter_context(nc.semaphore("s_dma%d" % k))
        self.dma_use = [0] * ndma
        self.dma_rr = 0
        self.out_events = []

    def _deps(self, e, reads, writes):
        ev = {}

        def add(x):
            if x is None:
                return
            k, v = x
            if ev.get(k, 0) < v:
                ev[k] = v
        for r in reads:
            add(self.lastw.get(r))
        for w in writes:
            add(self.lastw.get(w))
            for k, v in self.readers.get(w, {}).items():
                add((k, v))
        waits = []
        for k, v in ev.items():
            if k == e and e == "pe":
                continue
            if self.seen[e].get(k, 0) >= v:
                continue
            self.seen[e][k] = v
            waits.append((k, v))
        return waits

    def _record(self, ev, reads, writes):
        k, v = ev
        for r in reads:
            d = self.readers.setdefault(r, {})
            if d.get(k, 0) < v:
                d[k] = v
        for w in writes:
            self.lastw[w] = ev
            self.readers[w] = {}

    def op(self, e, fn, reads=(), writes=(), inc=True):
        waits = self._deps(e, reads, writes)
        if inc:
            self.cnt[e] += 1
            idx = self.cnt[e]
        else:
            idx = self.cnt[e] + 1
        self._record((e, idx), reads, writes)
        sem = self.sem[e]
        sems = self.sem

        def run(eng):
            for (k, v) in waits:
                eng.wait_ge(sems[k], v)
            ins = fn(eng)
            if inc:
                ins.then_inc(sem, 1)
        self.ops[e].append(run)

    def dma(self, q, fn, reads=(), writes=(), is_out=False):
        k = self.dma_rr
        self.dma_rr = (k + 1) % self.ndma
        waits = self._deps(q, reads, writes)
        key = ("dma", k)
        if self.dma_use[k] > 0:
            v = 16 * self.dma_use[k]
            if self.seen[q].get(key, 0) < v:
                self.seen[q][key] = v
                waits.append((key, v))
        self.dma_use[k] += 1
        ev = (key, 16 * self.dma_use[k])
        self._record(ev, reads, writes)
        if is_out:
            self.out_events.append(ev)
        sem = self.sem[key]
        sems = self.sem

        def run(eng):
            for (kk, v) in waits:
                eng.wait_ge(sems[kk], v)
            fn(eng).then_inc(sem, 16)
        self.ops[q].append(run)

    def finish(self):
        evs = {}
        for k, v in self.out_events:
            evs[k] = max(evs.get(k, 0), v)
        sems = self.sem
        lst = list(evs.items())

        def run(eng):
            for (k, v) in lst:
                eng.wait_ge(sems[k], v)
        self.ops["sp"].append(run)

    def emit(self):
        nc = self.nc
        ops = self.ops
        with nc.Block() as block:
            @block.tensor
            def _(eng):
                for f in ops["pe"]:
                    f(eng)

            @block.scalar
            def _(eng):
                for f in ops["act"]:
                    f(eng)

            @block.vector
            def _(eng):
                for f in ops["dve"]:
                    f(eng)

            @block.gpsimd
            def _(eng):
                for f in ops["pool"]:
                    f(eng)

            @block.sync
            def _(eng):
                for f in ops["sp"]:
                    f(eng)


def mm_group(S, out, pairs, reads, writes):
    n = len(pairs)
    for i, (lhsT, rhs) in enumerate(pairs):
        last = i == n - 1

        def fn(eng, lhsT=lhsT, rhs=rhs, i=i, last=last):
            return eng.matmul(out, lhsT, rhs, start=(i == 0), stop=last)
        S.op("pe", fn, reads=reads, writes=writes, inc=last)


def build_stage_a(T, layer):
    NB = T // TB
    nc = bass.Bass("TRN2", target_bir_lowering=False)
    xT = nc.dram_tensor("xT", [D, T], F32, kind="ExternalInput").ap()
    w_in = nc.dram_tensor("w_in", [D, 4 * D], F32, kind="ExternalInput").ap()
    w_out = nc.dram_tensor("w_out", [D, D], F32, kind="ExternalInput").ap()
    nw_d = nc.dram_tensor("nw", [128, NCH], F32, kind="ExternalInput").ap()
    onw_d = nc.dram_tensor("onw", [128, NH], F32, kind="ExternalInput").ap()
    lb0_d = nc.dram_tensor("lbl0", [128, NH], F32, kind="ExternalInput").ap()
    lb1_d = nc.dram_tensor("lbl1", [128, NH], F32, kind="ExternalInput").ap()
    s_in = nc.dram_tensor("s_in", [NH, 128, 128], F32, kind="ExternalInput").ap()
    ones_d = nc.dram_tensor("ones", [128, 128], BF16, kind="ExternalInput").ap()
    ident_d = nc.dram_tensor("ident", [128, 128], BF16, kind="ExternalInput").ap()
    cmask_d = nc.dram_tensor("cmask", [128, 128], F32, kind="ExternalInput").ap()
    smask_d = nc.dram_tensor("smask", [128, TB], F32, kind="ExternalInput").ap()
    xT_out = nc.dram_tensor("xT_out", [D, T], F32, kind="ExternalOutput").ap()
    s_out = nc.dram_tensor("s_out", [NH, 128, 128], F32, kind="ExternalOutput").ap()

    with contextlib.ExitStack() as st:
        def sb(name, shape, dt):
            return st.enter_context(nc.sbuf_tensor(name, shape, dt))

        def ps(name, shape, dt):
            return st.enter_context(nc.psum_tensor(name, shape, dt))

        hT = sb("hT", [128, NCH, T], BF16)
        oT = sb("oT", [128, NH, T], BF16)
        wbuf = [sb("wbuf%d" % i, [128, 4, NCH, 128], BF16) for i in range(2)]
        xs = [sb("xs%d" % i, [128, TB], F32) for i in range(2)]
        xsq = [sb("xsq%d" % i, [128, TB], BF16) for i in range(2)]
        t_sig = sb("t_sig", [128, TB], F32)
        t_g = sb("t_g", [128, TB], F32)
        t_b = sb("t_b", [128, TB], F32)
        t_eb = sb("t_eb", [128, TB], F32)
        t_enb = sb("t_enb", [128, TB], F32)
        t_sg = sb("t_sg", [128, TB], F32)
        t_rstd = sb("t_rstd", [128, TB], F32)
        t_t1 = sb("t_t1", [128, TB], F32)
        qtl = sb("qtl", [128, TB], BF16)
        ktl = sb("ktl", [128, TB], BF16)
        khf = sb("khf", [128, TB], BF16)
        osq = sb("osq", [128, TB], BF16)
        khT = sb("khT", [128, 4, 128], BF16)
        vtm = sb("vtm", [128, 4, 128], BF16)
        Am = sb("Am", [128, 128], BF16)
        Sst = sb("Sst", [128, 128], F32)
        Sbf = sb("Sbf", [128, 128], BF16)
        ones = sb("ones_sb", [128, 128], BF16)
        ident = sb("ident_sb", [128, 128], BF16)
        cmask = sb("cmask_sb", [128, 128], F32)
        smask = sb("smask_sb", [128, TB], F32)
        nw = sb("nw_sb", [128, NCH], F32)
        onw = sb("onw_sb", [128, NH], F32)
        lb = sb("lb_sb", [128, NH], F32)
        oml = sb("oml_sb", [128, NH], F32)
        lbt = sb("lbt_sb", [128, NH], F32)

        pq = ps("pq", [128, TB], F32)
        pf = ps("pf", [128, TB], F32)
        pg = ps("pg", [128, TB], F32)
        pv = ps("pv", [128, 4, 128], F32)
        po = ps("po", [128, TB], F32)
        pss = ps("pss", [128, TB], F32)
        pmisc = ps("pmisc", [128, TB], F32)
        pT = ps("pT", [128, 4, 128], BF16)
        pA = pmisc[:, 0:128]
        pU = pmisc[:, 128:256]

        S = Sched(nc, st)

        for (dst, src, nm) in ((ones, ones_d, "ones"), (ident, ident_d, "ident"),
                               (cmask, cmask_d, "cmask"), (smask, smask_d, "smask"),
                               (nw, nw_d, "nw"), (onw, onw_d, "onw"),
                               (lb, lb1_d, "lb"), (lbt, lb0_d, "lbt")):
            S.dma("sp", lambda e, dst=dst, src=src: e.dma_start(out=dst[:], in_=src), writes=[nm])
        if layer == 0:
            S.op("dve", lambda e: e.memset(lb[:], 0.0), writes=["lb"])
            S.op("dve", lambda e: e.memset(oml[:], 1.0), writes=["oml"])
        else:
            S.op("dve", lambda e: e.tensor_tensor(out=lb[:], in0=lb[:], in1=lbt[:], op=ALU.subtract),
                 reads=["lb", "lbt"], writes=["lb"])
            S.op("act", lambda e: e.activation(out=oml[:], in_=lb[:], func=AF.Sigmoid, scale=-1.0),
                 reads=["lb"], writes=["oml"])
            S.op("act", lambda e: e.activation(out=lb[:], in_=lb[:], func=AF.Sigmoid),
                 reads=["lb", "oml"], writes=["lb"])

        for tb in range(NB):
            tsl = slice(tb * TB, (tb + 1) * TB)
            for c in range(NCH):
                i = c % 2
                S.dma("sp", lambda e, i=i, c=c, tsl=tsl: e.dma_start(out=xs[i][:], in_=xT[c * 128:(c + 1) * 128, tsl]),
                      writes=["xs%d" % i])
                S.op("act", lambda e, i=i: e.activation(out=xsq[i][:], in_=xs[i][:], func=AF.Square),
                     reads=["xs%d" % i], writes=["xsq%d" % i])
                S.op("pe", lambda e, i=i, c=c: e.matmul(pss[:], ones[:], xsq[i][:], start=(c == 0), stop=(c == NCH - 1)),
                     reads=["xsq%d" % i, "ones"], writes=["pss"], inc=True)
            S.op("dve", lambda e: e.tensor_scalar(out=t_rstd[:], in0=pss[:], scalar1=1.0 / D, scalar2=EPS,
                                                  op0=ALU.mult, op1=ALU.add),
                 reads=["pss"], writes=["rstd"])
            S.op("act", lambda e: e.activation(out=t_rstd[:], in_=t_rstd[:], func=AF.Ln),
                 reads=["rstd"], writes=["rstd"])
            S.op("act", lambda e: e.activation(out=t_rstd[:], in_=t_rstd[:], func=AF.Exp, scale=-0.5),
                 reads=["rstd"], writes=["rstd"])
            for c in range(NCH):
                i = c % 2
                S.dma("sp", lambda e, i=i, c=c, tsl=tsl: e.dma_start(out=xs[i][:], in_=xT[c * 128:(c + 1) * 128, tsl]),
                      writes=["xs%d" % i])
                S.op("dve", lambda e, i=i, c=c, tsl=tsl: e.scalar_tensor_tensor(
                    out=hT[:, c, tsl], in0=xs[i][:], scalar=nw[:, c:c + 1], in1=t_rstd[:],
                    op0=ALU.mult, op1=ALU.mult),
                    reads=["xs%d" % i, "rstd", "nw"], writes=["hT%d" % tb])

        def load_head_w(h):
            wb = wbuf[h % 2]
            for j in range(4):
                col = j * D + h * 128
                src = w_in[:, col:col + 128].rearrange("(c p) n -> p c n", p=128)
                S.dma("pool", lambda e, wb=wb, j=j, src=src: e.dma_start(out=wb[:, j, :, :], in_=src),
                      writes=["wbuf%d" % (h % 2)])

        load_head_w(0)
        for h in range(NH):
            if h + 1 < NH:
                load_head_w(h + 1)
            wb = wbuf[h % 2]
            wname = "wbuf%d" % (h % 2)
            S.dma("sp", lambda e, h=h: e.dma_start(out=Sst[:], in_=s_in[h]), writes=["Sst"])
            S.op("act", lambda e: e.activation(out=Sbf[:], in_=Sst[:], func=AF.Copy),
                 reads=["Sst"], writes=["Sbf"])
            for tb in range(NB):
                tsl = slice(tb * TB, (tb + 1) * TB)
                hname = "hT%d" % tb
                for (pt, j, nm) in ((pq, 0, "pq"), (pf, 1, "pf"), (pg, 3, "pg")):
                    mm_group(S, pt[:], [(wb[:, j, c, :], hT[:, c, tsl]) for c in range(NCH)],
                             reads=[wname, hname], writes=[nm])
                for sub in range(4):
                    t0 = tb * TB + sub * 128
                    mm_group(S, pv[:, sub, :], [(hT[:, c, t0:t0 + 128], wb[:, 2, c, :]) for c in range(NCH)],
                             reads=[wname, hname], writes=["pv"])
                S.op("act", lambda e: e.activation(out=t_sig[:], in_=pf[:], func=AF.Sigmoid),
                     reads=["pf"], writes=["sig"])
                S.op("dve", lambda e, h=h: e.tensor_scalar(out=t_sig[:], in0=t_sig[:], scalar1=oml[:, h:h + 1],
                                                           scalar2=lb[:, h:h + 1], op0=ALU.mult, op1=ALU.add),
                     reads=["sig", "oml", "lb"], writes=["sig"])
                S.op("act", lambda e: e.activation(out=t_g[:], in_=t_sig[:], func=AF.Ln),
                     reads=["sig"], writes=["g"])
                S.op("dve", lambda e: e.tensor_tensor_scan(out=t_b[:], data0=smask[:], data1=t_g[:], initial=0.0,
                                                           op0=ALU.mult, op1=ALU.add),
                     reads=["g", "smask"], writes=["b"])
                S.op("act", lambda e: e.activation(out=t_eb[:], in_=t_b[:], func=AF.Exp),
                     reads=["b"], writes=["eb"])
                S.op("act", lambda e: e.activation(out=t_enb[:], in_=t_b[:], func=AF.Exp, scale=-1.0),
                     reads=["b"], writes=["enb"])
                S.op("dve", lambda e: e.tensor_scalar(out=t_sig[:], in0=t_sig[:], scalar1=-1.0, scalar2=1.0,
                                                      op0=ALU.mult, op1=ALU.add),
                     reads=["sig", "g"], writes=["sig"])
                S.op("dve", lambda e: e.tensor_tensor(out=qtl[:], in0=pq[:], in1=t_eb[:], op=ALU.mult),
                     reads=["pq", "eb"], writes=["qtl"])
                S.op("dve", lambda e: e.tensor_tensor(out=ktl[:], in0=t_sig[:], in1=t_enb[:], op=ALU.mult),
                     reads=["sig", "enb"], writes=["ktl"])
                b3 = t_b[:].rearrange("p (c k) -> p c k", k=CH)
                g3 = t_g[:].rearrange("p (c k) -> p c k", k=CH)
                S.op("dve", lambda e, b3=b3, g3=g3: e.tensor_tensor(
                    out=g3, in0=b3[:, :, CH - 1:CH].broadcast_to([128, TB // CH, CH]), in1=b3, op=ALU.subtract),
                    reads=["b", "g"], writes=["g"])
                S.op("act", lambda e: e.activation(out=t_g[:], in_=t_g[:], func=AF.Exp),
                     reads=["g"], writes=["g"])
                S.op("dve", lambda e: e.tensor_tensor(out=khf[:], in0=t_sig[:], in1=t_g[:], op=ALU.mult),
                     reads=["sig", "g"], writes=["khf"])
                S.op("act", lambda e: e.activation(out=t_sg[:], in_=pg[:], func=AF.Silu),
                     reads=["pg"], writes=["sg"])
                S.op("act", lambda e: e.activation(out=vtm[:], in_=pv[:], func=AF.Copy),
                     reads=["pv"], writes=["vtm"])
                for sub in range(4):
                    S.op("pe", lambda e, sub=sub: e.transpose(pT[:, sub, :], khf[:, sub * 128:(sub + 1) * 128], ident[:]),
                         reads=["khf", "ident"], writes=["pT"], inc=(sub == 3))
                S.op("act", lambda e: e.activation(out=khT[:], in_=pT[:], func=AF.Copy),
                     reads=["pT"], writes=["khT"])
                for sub in range(4):
                    ssl = slice(sub * 128, (sub + 1) * 128)
                    S.op("pe", lambda e, ssl=ssl: e.matmul(pA, ktl[:, ssl], qtl[:, ssl], start=True, stop=True),
                         reads=["ktl", "qtl"], writes=["pA"])
                    S.op("dve", lambda e: e.tensor_tensor(out=Am[:], in0=pA, in1=cmask[:], op=ALU.mult),
                         reads=["pA", "cmask"], writes=["Am"])
                    for c in range(4):
                        c0 = sub * 128 + c * CH
                        csl = slice(c0, c0 + CH)
                        S.op("pe", lambda e, csl=csl: e.matmul(po[:, csl], Sbf[:], qtl[:, csl], start=True, stop=False),
                             reads=["Sbf", "qtl"], writes=["po"], inc=False)
                        S.op("pe", lambda e, csl=csl, sub=sub, c=c: e.matmul(
                            po[:, csl], vtm[:, sub, :], Am[:, c * CH:(c + 1) * CH], start=False, stop=True),
                            reads=["vtm", "Am"], writes=["po"])
                        S.op("pe", lambda e, sub=sub, c=c: e.matmul(
                            pU, khT[c * CH:(c + 1) * CH, sub, :], vtm[c * CH:(c + 1) * CH, sub, :],
                            start=True, stop=True, tile_position=(c * CH, 0)),
                            reads=["khT", "vtm"], writes=["pU"])
                        S.op("dve", lambda e, c0=c0: e.scalar_tensor_tensor(
                            out=Sst[:], in0=Sst[:], scalar=t_eb[:, c0 + CH - 1:c0 + CH], in1=pU,
                            op0=ALU.mult, op1=ALU.add),
                            reads=["Sst", "eb", "pU"], writes=["Sst"])
                        S.op("act", lambda e: e.activation(out=Sbf[:], in_=Sst[:], func=AF.Copy),
                             reads=["Sst"], writes=["Sbf"])
                S.op("act", lambda e: e.activation(out=osq[:], in_=po[:], func=AF.Square),
                     reads=["po"], writes=["osq"])
                S.op("pe", lambda e: e.matmul(pss[:], ones[:], osq[:], start=True, stop=True),
                     reads=["osq", "ones"], writes=["pss"])
                S.op("dve", lambda e: e.tensor_scalar(out=t_rstd[:], in0=pss[:], scalar1=1.0 / 128, scalar2=EPS,
                                                      op0=ALU.mult, op1=ALU.add),
                     reads=["pss"], writes=["rstd"])
                S.op("act", lambda e: e.activation(out=t_rstd[:], in_=t_rstd[:], func=AF.Ln),
                     reads=["rstd"], writes=["rstd"])
                S.op("act", lambda e: e.activation(out=t_rstd[:], in_=t_rstd[:], func=AF.Exp, scale=-0.5),
                     reads=["rstd"], writes=["rstd"])
                S.op("dve", lambda e, h=h: e.scalar_tensor_tensor(
                    out=t_t1[:], in0=po[:], scalar=onw[:, h:h + 1], in1=t_rstd[:], op0=ALU.mult, op1=ALU.mult),
                    reads=["po", "onw", "rstd"], writes=["t1"])
                S.op("dve", lambda e, h=h, tsl=tsl: e.tensor_tensor(out=oT[:, h, tsl], in0=t_t1[:], in1=t_sg[:],
                                                                      op=ALU.mult),
                     reads=["t1", "sg"], writes=["oT%d" % tb])
            S.dma("sp", lambda e, h=h: e.dma_start(out=s_out[h], in_=Sst[:]), reads=["Sst"], is_out=True)

        def load_wo(j):
            wb = wbuf[j % 2]
            src = w_out[:, j * 128:(j + 1) * 128].rearrange("(h p) n -> p h n", p=128)
            S.dma("pool", lambda e, wb=wb, src=src: e.dma_start(out=wb[:, 0, :, :], in_=src),
                  writes=["wbuf%d" % (j % 2)])

        load_wo(0)
        for j in range(NCH):
            if j + 1 < NCH:
                load_wo(j + 1)
            wb = wbuf[j % 2]
            for tb in range(NB):
                tsl = slice(tb * TB, (tb + 1) * TB)
                i = tb % 2
                S.dma("sp", lambda e, i=i, j=j, tsl=tsl: e.dma_start(out=xs[i][:], in_=xT[j * 128:(j + 1) * 128, tsl]),
                      writes=["xs%d" % i])
                mm_group(S, pq[:], [(wb[:, 0, h, :], oT[:, h, tsl]) for h in range(NH)],
                         reads=["wbuf%d" % (j % 2), "oT%d" % tb], writes=["pq"])
                S.op("dve", lambda e, i=i: e.tensor_tensor(out=xs[i][:], in0=pq[:], in1=xs[i][:], op=ALU.add),
                     reads=["pq", "xs%d" % i], writes=["xs%d" % i])
                S.dma("sp", lambda e, i=i, j=j, tsl=tsl: e.dma_start(out=xT_out[j * 128:(j + 1) * 128, tsl], in_=xs[i][:]),
                      reads=["xs%d" % i], is_out=True)
        S.finish()
        S.emit()
    return nc


def consts():
    s = np.arange(128)[:, None]
    t = np.arange(128)[None, :]
    cmask = ((s // CH == t // CH) & (s <= t)).astype(np.float32)
    smask = np.ones((128, TB), np.float32)
    smask[:, ::CH] = 0.0
    return {
        "ones": np.ones((128, 128), NBF),
        "ident": np.eye(128, dtype=np.float32).astype(NBF),
        "cmask": cmask,
        "smask": smask,
    }


def chunked(v, n):
    return np.ascontiguousarray(v.reshape(n, 128).T)


def emit_norm_phase(S, T, xT, xs, xsq, pss, ones, t_rstd, nw, hT):
    NB = T // TB
    for tb in range(NB):
        tsl = slice(tb * TB, (tb + 1) * TB)
        for c in range(NCH):
            i = c % 2
            S.dma("sp", lambda e, i=i, c=c, tsl=tsl: e.dma_start(out=xs[i][:], in_=xT[c * 128:(c + 1) * 128, tsl]),
                  writes=["xs%d" % i])
            S.op("act", lambda e, i=i: e.activation(out=xsq[i][:], in_=xs[i][:], func=AF.Square),
                 reads=["xs%d" % i], writes=["xsq%d" % i])
            S.op("pe", lambda e, i=i, c=c: e.matmul(pss[:], ones[:], xsq[i][:], start=(c == 0), stop=(c == NCH - 1)),
                 reads=["xsq%d" % i, "ones"], writes=["pss"], inc=True)
        S.op("dve", lambda e: e.tensor_scalar(out=t_rstd[:], in0=pss[:], scalar1=1.0 / D, scalar2=EPS,
                                              op0=ALU.mult, op1=ALU.add),
             reads=["pss"], writes=["rstd"])
        S.op("act", lambda e: e.activation(out=t_rstd[:], in_=t_rstd[:], func=AF.Ln),
             reads=["rstd"], writes=["rstd"])
        S.op("act", lambda e: e.activation(out=t_rstd[:], in_=t_rstd[:], func=AF.Exp, scale=-0.5),
             reads=["rstd"], writes=["rstd"])
        for c in range(NCH):
            i = c % 2
            S.dma("sp", lambda e, i=i, c=c, tsl=tsl: e.dma_start(out=xs[i][:], in_=xT[c * 128:(c + 1) * 128, tsl]),
                  writes=["xs%d" % i])
            S.op("dve", lambda e, i=i, c=c, tsl=tsl: e.scalar_tensor_tensor(
                out=hT[:, c, tsl], in0=xs[i][:], scalar=nw[:, c:c + 1], in1=t_rstd[:],
                op0=ALU.mult, op1=ALU.mult),
                reads=["xs%d" % i, "rstd", "nw"], writes=["hT%d" % tb])


def emit_headnorm_rstd(S, src_ps, src_name, osq, pss, ones, t_rstd, mul, add):
    S.op("act", lambda e: e.activation(out=osq[:], in_=src_ps, func=AF.Square),
         reads=[src_name], writes=["osq"])
    S.op("pe", lambda e: e.matmul(pss[:], ones[:], osq[:], start=True, stop=True),
         reads=["osq", "ones"], writes=["pss"])
    S.op("dve", lambda e: e.tensor_scalar(out=t_rstd[:], in0=pss[:], scalar1=mul, scalar2=add,
                                          op0=ALU.mult, op1=ALU.add),
         reads=["pss"], writes=["rstd"])
    S.op("act", lambda e: e.activation(out=t_rstd[:], in_=t_rstd[:], func=AF.Ln),
         reads=["rstd"], writes=["rstd"])
    S.op("act", lambda e: e.activation(out=t_rstd[:], in_=t_rstd[:], func=AF.Exp, scale=-0.5),
         reads=["rstd"], writes=["rstd"])


def build_stage_kv(T):
    NB = T // TB
    nc = bass.Bass("TRN2", target_bir_lowering=False)
    xT = nc.dram_tensor("xT", [D, T], F32, kind="ExternalInput").ap()
    kv_w = nc.dram_tensor("kv_w", [D, 2 * D + NH], F32, kind="ExternalInput").ap()
    nw_d = nc.dram_tensor("nw", [128, NCH], F32, kind="ExternalInput").ap()
    knw_d = nc.dram_tensor("knw", [128, 1], F32, kind="ExternalInput").ap()
    fb_d = nc.dram_tensor("fb", [NH, 1], F32, kind="ExternalInput").ap()
    ones_d = nc.dram_tensor("ones", [128, 128], BF16, kind="ExternalInput").ap()
    kT_o = nc.dram_tensor("kT", [NH, 128, T], BF16, kind="ExternalOutput").ap()
    v_o = nc.dram_tensor("v", [NH, T, 128], BF16, kind="ExternalOutput").ap()
    faq_o = nc.dram_tensor("faq", [NH, 6, T], BF16, kind="ExternalOutput").ap()
    fako_o = nc.dram_tensor("fak_own", [NH, 6, T], BF16, kind="ExternalOutput").ap()
    fakp_o = nc.dram_tensor("fak_prev", [NH, 6, T], BF16, kind="ExternalOutput").ap()

    with contextlib.ExitStack() as st:
        def sb(name, shape, dt):
            return st.enter_context(nc.sbuf_tensor(name, shape, dt))

        def ps(name, shape, dt):
            return st.enter_context(nc.psum_tensor(name, shape, dt))

        hT = sb("hT", [128, NCH, T], BF16)
        wbuf = [sb("wbuf%d" % i, [128, 2, NCH, 128], BF16) for i in range(2)]
        wf = sb("wf", [128, NCH, NH], BF16)
        xs = [sb("xs%d" % i, [128, TB], F32) for i in range(2)]
        xsq = [sb("xsq%d" % i, [128, TB], BF16) for i in range(2)]
        t_rstd = sb("t_rstd", [128, TB], F32)
        osq = sb("osq", [128, TB], BF16)
        kst = [sb("kst%d" % i, [128, TB], BF16) for i in range(2)]
        vst = [sb("vst%d" % i, [128, 4, 128], BF16) for i in range(2)]
        ones = sb("ones_sb", [128, 128], BF16)
        nw = sb("nw_sb", [128, NCH], F32)
        knw = sb("knw_sb", [128, 1], F32)
        fb = sb("fb_sb", [NH, 1], F32)
        Fl = sb("Fl", [NH, T], F32)
        Gl = sb("Gl", [NH, T], F32)
        r1 = sb("r1", [NH, T], F32)
        onesT = sb("onesT", [NH, T], BF16)
        onesTf = sb("onesTf", [NH, T], F32)
        spl = [sb("spl%d" % i, [NH, T], BF16) for i in range(3)]

        pq = ps("pq", [128, TB], F32)
        pv = ps("pv", [128, 4, 128], F32)
        pss = ps("pss", [128, TB], F32)
        pfz = ps("pfz", [NH, TB], F32)

        S = Sched(nc, st)
        for (dst, src, nm) in ((ones, ones_d, "ones"), (nw, nw_d, "nw"), (knw, knw_d, "knw"), (fb, fb_d, "fb")):
            S.dma("sp", lambda e, dst=dst, src=src: e.dma_start(out=dst[:], in_=src), writes=[nm])
        S.op("dve", lambda e: e.memset(onesT[:], 1.0), writes=["onesT"])
        S.op("dve", lambda e: e.memset(onesTf[:], 1.0), writes=["onesTf"])
        src = kv_w[:, 2 * D:2 * D + NH].rearrange("(c p) n -> p c n", p=128)
        S.dma("pool", lambda e, src=src: e.dma_start(out=wf[:], in_=src), writes=["wf"])

        emit_norm_phase(S, T, xT, xs, xsq, pss, ones, t_rstd, nw, hT)

        for tb in range(NB):
            tsl = slice(tb * TB, (tb + 1) * TB)
            mm_group(S, pfz[:], [(wf[:, c, :], hT[:, c, tsl]) for c in range(NCH)],
                     reads=["wf", "hT%d" % tb], writes=["pfz"])
            S.op("act", lambda e, tsl=tsl: e.activation(out=r1[:, tsl], in_=pfz[:], func=AF.Sigmoid,
                                                         bias=fb[:, 0:1], scale=1.0),
                 reads=["pfz", "fb"], writes=["r1"])
            S.op("act", lambda e, tsl=tsl: e.activation(out=r1[:, tsl], in_=r1[:, tsl], func=AF.Ln),
                 reads=["r1"], writes=["r1"])
            if tb == 0:
                S.op("dve", lambda e, tsl=tsl: e.tensor_tensor_scan(
                    out=Fl[:, tsl], data0=onesTf[:, tsl], data1=r1[:, tsl], initial=0.0, op0=ALU.mult, op1=ALU.add),
                    reads=["r1", "onesTf"], writes=["Fl"])
            else:
                S.op("dve", lambda e, tsl=tsl, tb=tb: e.tensor_tensor_scan(
                    out=Fl[:, tsl], data0=onesTf[:, tsl], data1=r1[:, tsl],
                    initial=Fl[:, tb * TB - 1:tb * TB], op0=ALU.mult, op1=ALU.add),
                    reads=["r1", "onesTf", "Fl"], writes=["Fl"])
        S.op("dve", lambda e: e.tensor_scalar(out=Gl[:], in0=Fl[:], scalar1=Fl[:, T - 1:T], scalar2=None,
                                              op0=ALU.subtract),
             reads=["Fl"], writes=["Gl"])

        def split3(srcF, neg, dst, row0, tag):
            sgn = -1.0 if neg else 1.0
            S.op("dve", lambda e: e.tensor_scalar(out=r1[:], in0=srcF[:], scalar1=sgn, scalar2=None, op0=ALU.mult),
                 reads=[tag, "r1"], writes=["r1"])
            for i in range(3):
                S.op("dve", lambda e, i=i: e.tensor_copy(out=spl[i][:], in_=r1[:]),
                     reads=["r1"], writes=["spl%d" % i])
                if i < 2:
                    S.op("dve", lambda e, i=i: e.tensor_tensor(out=r1[:], in0=r1[:], in1=spl[i][:], op=ALU.subtract),
                         reads=["r1", "spl%d" % i], writes=["r1"])
                S.dma("sp", lambda e, i=i: e.dma_start(out=dst[:, row0 + i, :], in_=spl[i][:]),
                      reads=["spl%d" % i], is_out=True)

        split3(Fl, False, faq_o, 0, "Fl")
        split3(Fl, True, fako_o, 3, "Fl")
        split3(Gl, True, fakp_o, 3, "Gl")
        for (dst, row0) in ((faq_o, 3), (fako_o, 0), (fakp_o, 0)):
            for i in range(3):
                S.dma("sp", lambda e, dst=dst, row0=row0, i=i: e.dma_start(out=dst[:, row0 + i, :], in_=onesT[:]),
                      reads=["onesT"], is_out=True)

        def load_w(h):
            wb = wbuf[h % 2]
            for j in range(2):
                col = j * D + h * 128
                src = kv_w[:, col:col + 128].rearrange("(c p) n -> p c n", p=128)
                S.dma("pool", lambda e, wb=wb, j=j, src=src: e.dma_start(out=wb[:, j, :, :], in_=src),
                      writes=["wbuf%d" % (h % 2)])

        load_w(0)
        it = 0
        for h in range(NH):
            if h + 1 < NH:
                load_w(h + 1)
            wb = wbuf[h % 2]
            wname = "wbuf%d" % (h % 2)
            for tb in range(NB):
                tsl = slice(tb * TB, (tb + 1) * TB)
                hname = "hT%d" % tb
                i = it % 2
                it += 1
                mm_group(S, pq[:], [(wb[:, 0, c, :], hT[:, c, tsl]) for c in range(NCH)],
                         reads=[wname, hname], writes=["pq"])
                for sub in range(4):
                    t0 = tb * TB + sub * 128
                    mm_group(S, pv[:, sub, :], [(hT[:, c, t0:t0 + 128], wb[:, 1, c, :]) for c in range(NCH)],
                             reads=[wname, hname], writes=["pv"])
                emit_headnorm_rstd(S, pq[:], "pq", osq, pss, ones, t_rstd, 1.0 / 128, EPS)
                S.op("dve", lambda e, i=i: e.scalar_tensor_tensor(
                    out=kst[i][:], in0=pq[:], scalar=knw[:, 0:1], in1=t_rstd[:], op0=ALU.mult, op1=ALU.mult),
                    reads=["pq", "knw", "rstd"], writes=["kst%d" % i])
                S.dma("sp", lambda e, i=i, h=h, tsl=tsl: e.dma_start(out=kT_o[h, :, tsl], in_=kst[i][:]),
                      reads=["kst%d" % i], is_out=True)
                S.op("act", lambda e, i=i: e.activation(out=vst[i][:], in_=pv[:], func=AF.Copy),
                     reads=["pv"], writes=["vst%d" % i])
                dstv = v_o[h, tb * TB:(tb + 1) * TB, :].rearrange("(s p) e -> p s e", p=128)
                S.dma("sp", lambda e, i=i, dstv=dstv: e.dma_start(out=dstv, in_=vst[i][:]),
                      reads=["vst%d" % i], is_out=True)
        S.finish()
        S.emit()
    return nc


def build_stage_b(T):
    NB = T // TB
    NSB = T // 128
    nc = bass.Bass("TRN2", target_bir_lowering=False)
    xT = nc.dram_tensor("xT", [D, T], F32, kind="ExternalInput").ap()
    w_in = nc.dram_tensor("w_in", [D, 2 * D], F32, kind="ExternalInput").ap()
    w_out = nc.dram_tensor("w_out", [D, D], F32, kind="ExternalInput").ap()
    nw_d = nc.dram_tensor("nw", [128, NCH], F32, kind="ExternalInput").ap()
    qnw_d = nc.dram_tensor("qnw", [128, 1], F32, kind="ExternalInput").ap()
    onw_d = nc.dram_tensor("onw", [128, NH], F32, kind="ExternalInput").ap()
    ones_d = nc.dram_tensor("ones", [128, 128], BF16, kind="ExternalInput").ap()
    dmask_d = nc.dram_tensor("dmask", [128, 4, TB], F32, kind="ExternalInput").ap()
    kT_d = [nc.dram_tensor(n, [NH, 128, T], BF16, kind="ExternalInput").ap() for n in ("kT_prev", "kT_own")]
    v_d = [nc.dram_tensor(n, [NH, T, 128], BF16, kind="ExternalInput").ap() for n in ("v_prev", "v_own")]
    fak_d = [nc.dram_tensor(n, [NH, 6, T], BF16, kind="ExternalInput").ap() for n in ("fak_prev", "fak_own")]
    faq_d = nc.dram_tensor("faq", [NH, 6, T], BF16, kind="ExternalInput").ap()
    xT_out = nc.dram_tensor("xT_out", [D, T], F32, kind="ExternalOutput").ap()

    with contextlib.ExitStack() as st:
        def sb(name, shape, dt):
            return st.enter_context(nc.sbuf_tensor(name, shape, dt))

        def ps(name, shape, dt):
            return st.enter_context(nc.psum_tensor(name, shape, dt))

        hT = sb("hT", [128, NCH, T], BF16)
        oT = sb("oT", [128, NH, T], BF16)
        wbuf = [sb("wbuf%d" % i, [128, 2, NCH, 128], BF16) for i in range(2)]
        kTs = [sb("kTs%d" % i, [128, T], BF16) for i in range(2)]
        vs = [sb("vs%d" % i, [128, NSB, 128], BF16) for i in range(2)]
        faks = [sb("faks%d" % i, [6, T], BF16) for i in range(2)]
        faqs = sb("faqs", [6, T], BF16)
        xs = [sb("xs%d" % i, [128, TB], F32) for i in range(2)]
        xsq = [sb("xsq%d" % i, [128, TB], BF16) for i in range(2)]
        t_rstd = sb("t_rstd", [128, TB], F32)
        t_sg = sb("t_sg", [128, TB], F32)
        t_t1 = sb("t_t1", [128, TB], F32)
        t_on = sb("t_on", [128, TB], F32)
        t_rd = sb("t_rd", [128, TB], F32)
        t_ms = sb("t_ms", [128, TB], F32)
        osq = sb("osq", [128, TB], BF16)
        qn = sb("qn", [128, TB], BF16)
        PT = [sb("PT%d" % i, [128, TB], BF16) for i in range(2)]
        ones = sb("ones_sb", [128, 128], BF16)
        dmask = sb("dmask_sb", [128, 4, TB], F32)
        nw = sb("nw_sb", [128, NCH], F32)
        qnw = sb("qnw_sb", [128, 1], F32)
        onw = sb("onw_sb", [128, NH], F32)

        pq = ps("pq", [128, TB], F32)
        pg = ps("pg", [128, TB], F32)
        pS = [ps("pS%d" % i, [128, TB], F32) for i in range(2)]
        po = ps("po", [128, TB], F32)
        pden = ps("pden", [128, TB], F32)
        pss = ps("pss", [128, TB], F32)

        S = Sched(nc, st)
        for (dst, src, nm) in ((ones, ones_d, "ones"), (nw, nw_d, "nw"), (qnw, qnw_d, "qnw"),
                               (onw, onw_d, "onw"), (dmask, dmask_d, "dmask")):
            S.dma("sp", lambda e, dst=dst, src=src: e.dma_start(out=dst[:], in_=src), writes=[nm])

        emit_norm_phase(S, T, xT, xs, xsq, pss, ones, t_rstd, nw, hT)

        def load_w(h):
            wb = wbuf[h % 2]
            for j in range(2):
                col = j * D + h * 128
                src = w_in[:, col:col + 128].rearrange("(c p) n -> p c n", p=128)
                S.dma("pool", lambda e, wb=wb, j=j, src=src: e.dma_start(out=wb[:, j, :, :], in_=src),
                      writes=["wbuf%d" % (h % 2)])

        load_w(0)
        pi = 0
        for h in range(NH):
            if h + 1 < NH:
                load_w(h + 1)
            wb = wbuf[h % 2]
            wname = "wbuf%d" % (h % 2)
            for i in range(2):
                S.dma("sp", lambda e, i=i, h=h: e.dma_start(out=kTs[i][:], in_=kT_d[i][h]), writes=["kTs%d" % i])
                srcv = v_d[i][h].rearrange("(s p) e -> p s e", p=128)
                S.dma("sp", lambda e, i=i, srcv=srcv: e.dma_start(out=vs[i][:], in_=srcv), writes=["vs%d" % i])
                S.dma("sp", lambda e, i=i, h=h: e.dma_start(out=faks[i][:], in_=fak_d[i][h]), writes=["faks%d" % i])
            S.dma("sp", lambda e, h=h: e.dma_start(out=faqs[:], in_=faq_d[h]), writes=["faqs"])
            for tb in range(NB):
                tsl = slice(tb * TB, (tb + 1) * TB)
                hname = "hT%d" % tb
                mm_group(S, pq[:], [(wb[:, 0, c, :], hT[:, c, tsl]) for c in range(NCH)],
                         reads=[wname, hname], writes=["pq"])
                mm_group(S, pg[:], [(wb[:, 1, c, :], hT[:, c, tsl]) for c in range(NCH)],
                         reads=[wname, hname], writes=["pg"])
                emit_headnorm_rstd(S, pq[:], "pq", osq, pss, ones, t_rstd, 1.0, 128 * EPS)
                S.op("dve", lambda e: e.scalar_tensor_tensor(
                    out=qn[:], in0=pq[:], scalar=qnw[:, 0:1], in1=t_rstd[:], op0=ALU.mult, op1=ALU.mult),
                    reads=["pq", "qnw", "rstd"], writes=["qn"])
                S.op("act", lambda e: e.activation(out=t_sg[:], in_=pg[:], func=AF.Silu),
                     reads=["pg"], writes=["sg"])
                blocks = [(0, sb_) for sb_ in range(NSB)] + [(1, sb_) for sb_ in range(4 * (tb + 1))]
                nblk = len(blocks)
                for bi, (half, sb_) in enumerate(blocks):
                    p = pi % 2
                    pi += 1
                    ssl = slice(sb_ * 128, (sb_ + 1) * 128)
                    psn = "pS%d" % p
                    S.op("pe", lambda e, p=p, half=half, ssl=ssl: e.matmul(
                        pS[p][:], kTs[half][:, ssl], qn[:], start=True, stop=False),
                        reads=["kTs%d" % half, "qn"], writes=[psn], inc=False)
                    S.op("pe", lambda e, p=p, half=half, ssl=ssl, tsl=tsl: e.matmul(
                        pS[p][:], faks[half][:, ssl], faqs[:, tsl], start=False, stop=True),
                        reads=["faks%d" % half, "faqs"], writes=[psn])
                    diag = half == 1 and sb_ >= 4 * tb
                    if diag:
                        j = sb_ - 4 * tb
                        S.op("dve", lambda e, p=p, j=j: e.tensor_tensor(out=t_ms[:], in0=pS[p][:], in1=dmask[:, j, :],
                                                                        op=ALU.add),
                             reads=[psn, "dmask"], writes=["ms"])
                        S.op("act", lambda e, p=p: e.activation(out=PT[p][:], in_=t_ms[:], func=AF.Exp),
                             reads=["ms"], writes=["PT%d" % p])
                    else:
                        S.op("act", lambda e, p=p: e.activation(out=PT[p][:], in_=pS[p][:], func=AF.Exp),
                             reads=[psn], writes=["PT%d" % p])
                    S.op("pe", lambda e, p=p, half=half, sb_=sb_, bi=bi: e.matmul(
                        po[:], vs[half][:, sb_, :], PT[p][:], start=(bi == 0), stop=(bi == nblk - 1)),
                        reads=["vs%d" % half, "PT%d" % p], writes=["po"], inc=(bi == nblk - 1))
                    S.op("pe", lambda e, p=p, bi=bi: e.matmul(
                        pden[:], ones[:], PT[p][:], start=(bi == 0), stop=(bi == nblk - 1)),
                        reads=["ones", "PT%d" % p], writes=["pden"], inc=True)
                S.op("dve", lambda e: e.reciprocal(out=t_rd[:], in_=pden[:]), reads=["pden"], writes=["rd"])
                S.op("dve", lambda e: e.tensor_tensor(out=t_on[:], in0=po[:], in1=t_rd[:], op=ALU.mult),
                     reads=["po", "rd"], writes=["on"])
                emit_headnorm_rstd(S, t_on[:], "on", osq, pss, ones, t_rstd, 1.0 / 128, EPS)
                S.op("dve", lambda e, h=h: e.scalar_tensor_tensor(
                    out=t_t1[:], in0=t_on[:], scalar=onw[:, h:h + 1], in1=t_rstd[:], op0=ALU.mult, op1=ALU.mult),
                    reads=["on", "onw", "rstd"], writes=["t1"])
                S.op("dve", lambda e, h=h, tsl=tsl: e.tensor_tensor(out=oT[:, h, tsl], in0=t_t1[:], in1=t_sg[:],
                                                                      op=ALU.mult),
                     reads=["t1", "sg"], writes=["oT%d" % tb])

        def load_wo(j):
            wb = wbuf[j % 2]
            src = w_out[:, j * 128:(j + 1) * 128].rearrange("(h p) n -> p h n", p=128)
            S.dma("pool", lambda e, wb=wb, src=src: e.dma_start(out=wb[:, 0, :, :], in_=src),
                  writes=["wbuf%d" % (j % 2)])

        load_wo(0)
        for j in range(NCH):
            if j + 1 < NCH:
                load_wo(j + 1)
            wb = wbuf[j % 2]
            for tb in range(NB):
                tsl = slice(tb * TB, (tb + 1) * TB)
                i = tb % 2
                S.dma("sp", lambda e, i=i, j=j, tsl=tsl: e.dma_start(out=xs[i][:], in_=xT[j * 128:(j + 1) * 128, tsl]),
                      writes=["xs%d" % i])
                mm_group(S, pq[:], [(wb[:, 0, h, :], oT[:, h, tsl]) for h in range(NH)],
                         reads=["wbuf%d" % (j % 2), "oT%d" % tb], writes=["pq"])
                S.op("dve", lambda e, i=i: e.tensor_tensor(out=xs[i][:], in0=pq[:], in1=xs[i][:], op=ALU.add),
                     reads=["pq", "xs%d" % i], writes=["xs%d" % i])
                S.dma("sp", lambda e, i=i, j=j, tsl=tsl: e.dma_start(out=xT_out[j * 128:(j + 1) * 128, tsl], in_=xs[i][:]),
                      reads=["xs%d" % i], is_out=True)
        S.finish()
        S.emit()
    return nc


def dmask_const():
    s = np.arange(128)[:, None, None]
    j = np.arange(4)[None, :, None]
    t = np.arange(TB)[None, None, :]
    return np.where(t >= 128 * j + s, 0.0, -30000.0).astype(np.float32)


_PROG = {}


def _prog(key, fn):
    if key not in _PROG:
        _PROG[key] = fn()
    return _PROG[key]


def _run(nc, in_maps):
    res = run_bass_kernel_spmd(nc, in_maps, core_ids=list(range(len(in_maps))))
    return res.results


def kernel(x, a_norm_w, a_w_in, a_lb_logits, a_out_norm_w, a_w_out,
           kv_norm_w, kv_w, kv_f_bias, k_norm_w,
           b_norm_w, b_w_in, b_q_norm_w, b_out_norm_w, b_w_out):
    f32 = lambda a: np.ascontiguousarray(np.asarray(a, dtype=np.float32))
    x = f32(x)
    B, SEQ, _ = x.shape
    T = SEQ // 2
    NC = 2 * B
    C = consts()
    a_norm_w, a_w_in, a_lb_logits = f32(a_norm_w), f32(a_w_in), f32(a_lb_logits)
    a_out_norm_w, a_w_out = f32(a_out_norm_w), f32(a_w_out)
    kv_norm_w, kv_w, kv_f_bias, k_norm_w = f32(kv_norm_w), f32(kv_w), f32(kv_f_bias), f32(k_norm_w)
    b_norm_w, b_w_in, b_q_norm_w = f32(b_norm_w), f32(b_w_in), f32(b_q_norm_w)
    b_out_norm_w, b_w_out = f32(b_out_norm_w), f32(b_w_out)

    xT = [np.ascontiguousarray(x[c // 2, (c % 2) * T:(c % 2 + 1) * T].T) for c in range(NC)]
    zeroS = np.zeros((NH, 128, 128), np.float32)

    for layer in range(2):
        nc = _prog(("a", T, layer), lambda: build_stage_a(T, layer))
        base = {"w_in": a_w_in[layer], "w_out": a_w_out[layer],
                "nw": chunked(a_norm_w[layer], NCH), "onw": chunked(a_out_norm_w[layer], NH),
                "lbl0": chunked(a_lb_logits[0], NH), "lbl1": chunked(a_lb_logits[1], NH)}
        base.update(C)
        r1 = _run(nc, [dict(base, xT=xT[c], s_in=zeroS) for c in range(NC)])
        r2 = _run(nc, [dict(base, xT=xT[c], s_in=(r1[c - 1]["s_out"] if c % 2 else zeroS)) for c in range(NC)])
        xT = [r2[c]["xT_out"] for c in range(NC)]

    nc = _prog(("kv", T), lambda: build_stage_kv(T))
    kv = _run(nc, [{"xT": xT[c], "kv_w": kv_w, "nw": chunked(kv_norm_w, NCH),
                    "knw": k_norm_w.reshape(128, 1).copy(), "fb": kv_f_bias.reshape(NH, 1).copy(),
                    "ones": C["ones"]} for c in range(NC)])
    fake_fak = np.zeros((NH, 6, T), NBF)
    fake_fak[:, 0:3] = 1
    fake_fak[:, 3] = -30000.0
    zk = np.zeros((NH, 128, T), NBF)
    zv = np.zeros((NH, T, 128), NBF)
    dm = dmask_const()

    nc = _prog(("b", T), lambda: build_stage_b(T))
    for j in range(2):
        ins = []
        for c in range(NC):
            d = {"xT": xT[c], "w_in": b_w_in[j], "w_out": b_w_out[j], "nw": chunked(b_norm_w[j], NCH),
                 "qnw": b_q_norm_w[j].reshape(128, 1).copy(), "onw": chunked(b_out_norm_w[j], NH),
                 "ones": C["ones"], "dmask": dm,
                 "kT_own": kv[c]["kT"], "v_own": kv[c]["v"], "fak_own": kv[c]["fak_own"], "faq": kv[c]["faq"]}
            if c % 2 == 0:
                d.update({"kT_prev": zk, "v_prev": zv, "fak_prev": fake_fak})
            else:
                d.update({"kT_prev": kv[c - 1]["kT"], "v_prev": kv[c - 1]["v"], "fak_prev": kv[c - 1]["fak_prev"]})
            ins.append(d)
        r = _run(nc, ins)
        xT = [r[c]["xT_out"] for c in range(NC)]

    out = np.empty((B, SEQ, D), np.float32)
    for c in range(NC):
        out[c // 2, (c % 2) * T:(c % 2 + 1) * T] = xT[c].T
    return out
```
